# Optimizing a Trainium2 kernel written in Bass

```python
import math
import jax, jax.numpy as jnp
from jax import lax
import numpy as np

D_MODEL = 1024
BATCH = 2
SEQ = 16384
DEPTH = 2

CHUNK = 64
N_MEM = 256
MLSTM_HEADS = 4
MLSTM_WIDTH = D_MODEL
MLSTM_HEAD_DIM = MLSTM_WIDTH // MLSTM_HEADS
QKV_BLOCK = 4
MLSTM_CONV = 4
CONV_WIDTH = D_MODEL
CONV_KERNEL = 31
XATTN_HEADS = 4
XATTN_HEAD_DIM = D_MODEL // XATTN_HEADS
D_FF = 2816
FFN_CONV = 3
RMS_EPS = 1e-6
LN_EPS = 1e-5
IN_COLS = 2 * MLSTM_WIDTH + 2 * CONV_WIDTH + 2 * D_MODEL

kernel_name = "hybrid_mlstm_conformer_conv_xattn_convffn"


def rmsnorm(x, g):
    xf = x.astype(jnp.float32)
    y = xf * lax.rsqrt(jnp.mean(xf * xf, -1, keepdims=True) + RMS_EPS)
    return (y * g.astype(jnp.float32)).astype(x.dtype)


def layernorm(x, g, b):
    xf = x.astype(jnp.float32)
    mu = jnp.mean(xf, -1, keepdims=True)
    var = jnp.mean(jnp.square(xf - mu), -1, keepdims=True)
    y = (xf - mu) * lax.rsqrt(var + LN_EPS)
    return (y * g.astype(jnp.float32) + b.astype(jnp.float32)).astype(x.dtype)


def causal_dwconv(x, w, b):
    K, C = w.shape
    y = lax.conv_general_dilated(
        x, w[:, None, :].astype(x.dtype), window_strides=(1,), padding=[(K - 1, 0)],
        dimension_numbers=("NWC", "WIO", "NWC"), feature_group_count=C)
    return y + b.astype(x.dtype)


def mlstm_chunkwise(q, k, v, i_pre, f_pre):
    B, H, S, dh = q.shape
    nc = S // CHUNK

    def to_chunks(t):
        t = t.reshape((B, H, nc, CHUNK) + t.shape[3:])
        return jnp.moveaxis(t, 2, 0)

    qc = to_chunks(q)
    kc = to_chunks(k * (dh ** -0.5))
    vc = to_chunks(v)
    ic = to_chunks(i_pre)
    lfc = to_chunks(jax.nn.log_sigmoid(f_pre))
    causal = jnp.tril(jnp.ones((CHUNK, CHUNK), dtype=bool))

    def step(carry, inp):
        C, n, m = carry
        q_, k_, v_, i_, lf_ = inp
        b = jnp.cumsum(lf_, axis=-1)
        a = b + m[..., None]
        dmat = b[..., :, None] - b[..., None, :] + i_[..., None, :]
        dmat = jnp.where(causal, dmat, -jnp.inf)
        m_row = jnp.maximum(a, jnp.max(dmat, axis=-1))
        w_inter = jnp.exp(a - m_row)
        s = jnp.einsum("bhld,bhsd->bhls", q_, k_) * jnp.exp(dmat - m_row[..., None])
        num = (jnp.einsum("bhls,bhsd->bhld", s, v_)
               + w_inter[..., None] * jnp.einsum("bhld,bhde->bhle", q_, C))
        den = jnp.sum(s, axis=-1) + w_inter * jnp.einsum("bhld,bhd->bhl", q_, n)
        h = num / jnp.maximum(jnp.abs(den), jnp.exp(-m_row))[..., None]
        b_end = b[..., -1]
        g = b_end[..., None] - b + i_
        m_new = jnp.maximum(b_end + m, jnp.max(g, axis=-1))
        decay = jnp.exp(b_end + m - m_new)
        wk = jnp.exp(g - m_new[..., None])[..., None] * k_
        C_new = decay[..., None, None] * C + jnp.einsum("bhld,bhle->bhde", wk, v_)
        n_new = decay[..., None] * n + jnp.sum(wk, axis=2)
        return (C_new, n_new, m_new), h

    init = (jnp.zeros((B, H, dh, dh), jnp.float32),
            jnp.zeros((B, H, dh), jnp.float32),
            jnp.zeros((B, H), jnp.float32))
    _, hc = lax.scan(step, init, (qc, kc, vc, ic, lfc))
    return jnp.moveaxis(hc, 0, 2).reshape(B, H, S, dh)


def mlstm_branch(xm, z, conv_w, conv_b, wq, wk, wv, w_gate, b_gate, norm_g, skip, w_down):
    B, S, W = xm.shape
    H, dh = MLSTM_HEADS, MLSTM_HEAD_DIM
    xc = jax.nn.silu(causal_dwconv(xm, conv_w, conv_b))

    def blockdiag(t, w):
        tb = t.reshape(B, S, W // QKV_BLOCK, QKV_BLOCK)
        return jnp.einsum("bsnc,ncd->bsnd", tb, w.astype(t.dtype)).reshape(B, S, W)

    q = blockdiag(xc, wq)
    k = blockdiag(xc, wk)
    v = blockdiag(xm, wv)
    gates = (jnp.concatenate([q, k, v], axis=-1) @ w_gate + b_gate).astype(jnp.float32)
    i_pre = jnp.transpose(gates[..., :H], (0, 2, 1))
    f_pre = jnp.transpose(gates[..., H:], (0, 2, 1))

    def heads(t):
        return jnp.transpose(t.reshape(B, S, H, dh), (0, 2, 1, 3)).astype(jnp.float32)

    h = mlstm_chunkwise(heads(q), heads(k), heads(v), i_pre, f_pre)
    mu = jnp.mean(h, -1, keepdims=True)
    var = jnp.mean(jnp.square(h - mu), -1, keepdims=True)
    hn = (h - mu) * lax.rsqrt(var + LN_EPS)
    hn = jnp.transpose(hn, (0, 2, 1, 3)).reshape(B, S, W).astype(xm.dtype) * norm_g
    out = (hn + skip * xc) * jax.nn.silu(z)
    return out @ w_down


def conformer_conv_branch(a, g, dw_w, dw_b, ln_g, ln_b, w_pw, b_pw):
    u = a * jax.nn.sigmoid(g)
    u = causal_dwconv(u, dw_w, dw_b)
    u = jax.nn.silu(layernorm(u, ln_g, ln_b))
    return u @ w_pw + b_pw


def cross_attention(h, mem_n, wq, wk, wv, wo):
    B, S, _ = h.shape
    N = mem_n.shape[1]
    q = (h @ wq).reshape(B, S, XATTN_HEADS, XATTN_HEAD_DIM)
    k = (mem_n @ wk).reshape(B, N, XATTN_HEADS, XATTN_HEAD_DIM)
    v = (mem_n @ wv).reshape(B, N, XATTN_HEADS, XATTN_HEAD_DIM)
    s = jnp.einsum("bshd,bnhd->bhsn", q, k).astype(jnp.float32) * (XATTN_HEAD_DIM ** -0.5)
    p = jax.nn.softmax(s, axis=-1).astype(v.dtype)
    o = jnp.einsum("bhsn,bnhd->bshd", p, v).reshape(B, S, D_MODEL)
    return o @ wo


def conv_ffn(h, w_up, b_up, dw_w, dw_b, w_down):
    u = causal_dwconv(h @ w_up + b_up, dw_w, dw_b)
    gate, val = u[..., :D_FF], u[..., D_FF:]
    return (jax.nn.silu(gate) * val) @ w_down


def setup_inputs(seed: int = 0) -> dict:
    key = jax.random.key(seed)
    ks = iter(jax.random.split(key, 48))

    def nrm(shape, scale):
        return jax.random.normal(next(ks), shape, jnp.float32) * scale

    L, D, W, CW, F, H = DEPTH, D_MODEL, MLSTM_WIDTH, CONV_WIDTH, D_FF, MLSTM_HEADS
    nb = W // QKV_BLOCK
    f_bias = jnp.broadcast_to(jnp.linspace(3.0, 6.0, H, dtype=jnp.float32), (L, H)) + nrm((L, H), 0.01)
    ml_b_gate = jnp.concatenate([nrm((L, H), 0.1), f_bias], axis=-1)
    return {
        "x": nrm((BATCH, SEQ, D), 1.0),
        "mem": nrm((BATCH, N_MEM, D), 1.0),
        "norm_mix": 1.0 + nrm((L, D), 0.02),
        "w_in": nrm((L, D, IN_COLS), D ** -0.5),
        "b_in": nrm((L, IN_COLS), 0.02),
        "ml_conv_w": nrm((L, MLSTM_CONV, W), MLSTM_CONV ** -0.5),
        "ml_conv_b": nrm((L, W), 0.02),
        "ml_wq": nrm((L, nb, QKV_BLOCK, QKV_BLOCK), QKV_BLOCK ** -0.5),
        "ml_wk": nrm((L, nb, QKV_BLOCK, QKV_BLOCK), QKV_BLOCK ** -0.5),
        "ml_wv": nrm((L, nb, QKV_BLOCK, QKV_BLOCK), QKV_BLOCK ** -0.5),
        "ml_w_gate": nrm((L, 3 * W, 2 * H), (3 * W) ** -0.5),
        "ml_b_gate": ml_b_gate,
        "ml_norm_g": 1.0 + nrm((L, W), 0.02),
        "ml_skip": 1.0 + nrm((L, W), 0.02),
        "ml_w_down": nrm((L, W, D), W ** -0.5),
        "cv_dw_w": nrm((L, CONV_KERNEL, CW), CONV_KERNEL ** -0.5),
        "cv_dw_b": nrm((L, CW), 0.02),
        "cv_ln_g": 1.0 + nrm((L, CW), 0.02),
        "cv_ln_b": nrm((L, CW), 0.02),
        "cv_w_pw": nrm((L, CW, D), CW ** -0.5),
        "cv_b_pw": nrm((L, D), 0.02),
        "w_out": nrm((L, D, D), D ** -0.5),
        "norm_x": 1.0 + nrm((L, D), 0.02),
        "norm_mem": 1.0 + nrm((L, D), 0.02),
        "xa_wq": nrm((L, D, D), D ** -0.5),
        "xa_wk": nrm((L, D, D), D ** -0.5),
        "xa_wv": nrm((L, D, D), D ** -0.5),
        "xa_wo": nrm((L, D, D), D ** -0.5),
        "norm_ffn": 1.0 + nrm((L, D), 0.02),
        "ffn_w_up": nrm((L, D, 2 * F), D ** -0.5),
        "ffn_b_up": nrm((L, 2 * F), 0.02),
        "ffn_dw_w": nrm((L, FFN_CONV, 2 * F), FFN_CONV ** -0.5),
        "ffn_dw_b": nrm((L, 2 * F), 0.02),
        "ffn_w_down": nrm((L, F, D), F ** -0.5),
        "final_norm": 1.0 + nrm((D,), 0.02),
    }


def reference(x, mem, norm_mix, w_in, b_in, ml_conv_w, ml_conv_b, ml_wq, ml_wk, ml_wv,
              ml_w_gate, ml_b_gate, ml_norm_g, ml_skip, ml_w_down, cv_dw_w, cv_dw_b,
              cv_ln_g, cv_ln_b, cv_w_pw, cv_b_pw, w_out, norm_x, norm_mem, xa_wq, xa_wk,
              xa_wv, xa_wo, norm_ffn, ffn_w_up, ffn_b_up, ffn_dw_w, ffn_dw_b, ffn_w_down,
              final_norm):
    W, CW, D = MLSTM_WIDTH, CONV_WIDTH, D_MODEL
    o1, o2, o3, o4, o5 = W, 2 * W, 2 * W + CW, 2 * W + 2 * CW, 2 * W + 2 * CW + D
    for l in range(DEPTH):
        h = rmsnorm(x, norm_mix[l])
        p = h @ w_in[l] + b_in[l]
        y_m = mlstm_branch(p[..., :o1], p[..., o1:o2], ml_conv_w[l], ml_conv_b[l],
                           ml_wq[l], ml_wk[l], ml_wv[l], ml_w_gate[l], ml_b_gate[l],
                           ml_norm_g[l], ml_skip[l], ml_w_down[l])
        y_c = conformer_conv_branch(p[..., o2:o3], p[..., o3:o4], cv_dw_w[l], cv_dw_b[l],
                                    cv_ln_g[l], cv_ln_b[l], cv_w_pw[l], cv_b_pw[l])
        merged = jax.nn.sigmoid(p[..., o4:o5]) * y_m + jax.nn.sigmoid(p[..., o5:]) * y_c
        x = x + merged @ w_out[l]
        x = x + cross_attention(rmsnorm(x, norm_x[l]), rmsnorm(mem, norm_mem[l]),
                                xa_wq[l], xa_wk[l], xa_wv[l], xa_wo[l])
        x = x + conv_ffn(rmsnorm(x, norm_ffn[l]), ffn_w_up[l], ffn_b_up[l],
                         ffn_dw_w[l], ffn_dw_b[l], ffn_w_down[l])
    return rmsnorm(x, final_norm)
```

```python
import math
import contextlib
import numpy as np
import concourse.bass as bass
import concourse.mybir as mybir
from concourse.bass_utils import run_bass_kernel_spmd

F32 = mybir.dt.float32
BF16 = mybir.dt.bfloat16
AF = mybir.ActivationFunctionType
ALU = mybir.AluOpType

D = 1024
KC = 8
NMEM = 256
NH = 4
DH = 256
DFF = 2816
FC = 44
HB = 32
NCORE = 8
GRP = 4
RMS_EPS = 1e-6
LN_EPS = 1e-5
LN16 = math.log(16.0)
SAME = True

VEC_LAYOUT = [('norm_mix', 8), ('b_in', 48), ('ml_conv_b', 8), ('ml_norm_g', 8), ('ml_skip', 8), ('cv_dw_b', 8),
              ('cv_ln_g', 8), ('cv_ln_b', 8), ('cv_b_pw', 8), ('norm_x', 8), ('norm_mem', 8), ('norm_ffn', 8),
              ('ffn_b_up', 44), ('ffn_dw_b', 44), ('ml_conv_w', 32), ('cv_dw_w', 248), ('ffn_dw_w', 132),
              ('b_gate', 8)]
VOFF = {}
_o = 0
for _n, _w in VEC_LAYOUT:
    VOFF[_n] = _o
    _o += _w
NVL = _o


class Op:
    pass


class Prog:
    ENGS = ('pe', 'act', 'dve', 'pool', 'sp')

    def __init__(s, nc, es):
        s.nc = nc
        s.es = es
        s.ops = {e: [] for e in s.ENGS}
        s.lastw = {}
        s.readers = {}
        s.dcount = {}
        s.sems = {}
        s.pending = {}
        s.muted = False

    def sem(s, key):
        if key not in s.sems:
            nm = "s_" + "".join(ch for ch in str(key) if ch.isalnum())
            s.sems[key] = s.es.enter_context(s.nc.semaphore(nm))
        return s.sems[key]

    def op(s, eng, fn, reads=(), writes=(), dkey=None, cc=False):
        if s.muted:
            return None
        o = Op()
        o.eng = eng
        o.idx = len(s.ops[eng])
        o.fn = fn
        o.needs_inc = False
        o.dkey = dkey
        o.cc = cc
        deps = []
        for r in reads:
            w = s.lastw.get(r)
            if w is not None:
                deps.append(w)
        for r in writes:
            w = s.lastw.get(r)
            if w is not None:
                deps.append(w)
            deps.extend(s.readers.get(r, {}).values())
        o.deps = list(s.pending.pop(eng, []))
        for d in deps:
            if d.dkey is not None:
                o.deps.append(('d', d.dkey, s.dcount[d.dkey]))
            else:
                o.deps.append(('e', d))
        if dkey is not None:
            s.dcount[dkey] = s.dcount.get(dkey, 0) + 1
        for r in reads:
            s.readers.setdefault(r, {})[dkey if dkey is not None else eng] = o
        for r in writes:
            s.lastw[r] = o
            s.readers[r] = {}
        s.ops[eng].append(o)
        return o

    def barrier(s):
        if s.muted:
            return
        deps = []
        for e in s.ENGS:
            for o in reversed(s.ops[e]):
                if o.dkey is None:
                    deps.append(('e', o))
                    break
        for k, v in s.dcount.items():
            deps.append(('d', k, v))
        for e in s.ENGS:
            s.pending[e] = s.pending.get(e, []) + deps
        s.lastw = {}
        s.readers = {}

    def emit(s, final_keys=()):
        nc = s.nc
        for e in s.ENGS:
            waited = {}
            for o in s.ops[e]:
                w = {}
                for d in o.deps:
                    if d[0] == 'd':
                        k = ('d', d[1])
                        if waited.get(k, 0) < d[2]:
                            waited[k] = d[2]
                            w[k] = d[2]
                    else:
                        x = d[1]
                        if x.eng == e and (e == 'pe' or not SAME):
                            continue
                        if x.eng == e and x.idx >= o.idx:
                            continue
                        if waited.get(x.eng, -1) < x.idx:
                            waited[x.eng] = x.idx
                            w[x.eng] = x
                o.waits = w
                for k, v in w.items():
                    if not isinstance(k, tuple):
                        v.needs_inc = True
        for e in s.ENGS:
            c = 0
            for o in s.ops[e]:
                if o.needs_inc:
                    c += 1
                o.val = c
        for e in s.ENGS:
            s.sem(e)
        for k in s.dcount:
            s.sem(('d', k))
        block = s.es.enter_context(nc.Block())

        def run(e, engobj):
            for o in s.ops[e]:
                for k, v in o.waits.items():
                    if isinstance(k, tuple):
                        inc = 1 if k[1].startswith('cc') else 16
                        engobj.wait_ge(s.sems[k], inc * v)
                    else:
                        engobj.wait_ge(s.sems[k], v.val)
                ins = o.fn(engobj)
                if o.dkey is not None:
                    ins.then_inc(s.sems[('d', o.dkey)], 1 if o.cc else 16)
                elif o.needs_inc:
                    ins.then_inc(s.sems[e], 1)
            if e == 'sp':
                for k in final_keys:
                    if k not in s.dcount:
                        continue
                    inc = 1 if k.startswith('cc') else 16
                    engobj.wait_ge(s.sems[('d', k)], inc * s.dcount[k])

        @block.tensor
        def _(eng):
            run('pe', eng)

        @block.scalar
        def _(eng):
            run('act', eng)

        @block.vector
        def _(eng):
            run('dve', eng)

        @block.gpsimd
        def _(eng):
            run('pool', eng)

        @block.sync
        def _(eng):
            run('sp', eng)


class _Stop(Exception):
    pass


class Cfg:
    def __init__(s, S=4096, T=256, L=2, debug=False, stop=None):
        s.stop = stop
        s.S = S
        s.T = T
        s.L = L
        s.debug = debug


def build(cfg):
    S, T, L = cfg.S, cfg.T, cfg.L
    NT = S // T
    NJ = T // 128
    nc = bass.Bass("TRN2", target_bir_lowering=False)
    es = contextlib.ExitStack()

    def din(name, shape):
        return nc.dram_tensor(name, shape, F32, kind="ExternalInput").ap()

    xT_in = din("xT", [D, S])
    memT_in = din("memT", [D, NMEM])
    w_in = din("w_in", [L, D, 6144])
    bd_in = din("bd", [L, 3, KC, 128, 128])
    wg_in = din("w_gate", [L, 128, 24 * 8])
    wmd_in = din("ml_w_down", [L, D, D])
    wpw_in = din("cv_w_pw", [L, D, D])
    wout_in = din("w_out", [L, D, D])
    xwq_in = din("xa_wq", [L, D, D])
    xwk_in = din("xa_wk", [L, D, D])
    xwv_in = din("xa_wv", [L, D, D])
    xwo_in = din("xa_wo", [L, D, D])
    wup_in = din("ffn_w_up", [L, D, 2 * DFF])
    wdn_in = din("ffn_w_down", [L, DFF, D])
    vecs_in = din("vecs", [128, L * NVL + 8])
    cst_in = din("consts", [128, 3 * 128])
    pc_in = din("pc", [128, 24])
    outT = nc.dram_tensor("outT", [D, S], F32, kind="ExternalOutput").ap()

    dk = "ExternalOutput" if cfg.debug else "Internal"
    D1 = nc.dram_tensor("D1", [D, S], F32, kind=dk).ap()
    D2 = nc.dram_tensor("D2", [D, S], F32, kind=dk).ap()
    D3 = nc.dram_tensor("D3", [D, S], F32, kind=dk).ap()
    D4 = nc.dram_tensor("D4", [D, S], F32, kind=dk).ap()
    GY = nc.dram_tensor("GY", [D, S], BF16, kind=dk).ap()
    UB = nc.dram_tensor("UB", [D, S], BF16, kind=dk).ap()
    SGCB = nc.dram_tensor("SGCB", [D, S], BF16, kind=dk).ap()
    hins = [nc.dram_tensor("hin%d" % i, [D, HB], F32) for i in range(2 * L)]
    houts = [nc.dram_tensor("hout%d" % i, [GRP * D, HB], F32) for i in range(2 * L)]
    stins = [[nc.dram_tensor("stin%d_%d" % (i, k), [D, 32], F32) for k in range(9)] for i in range(L)]
    stouts = [[nc.dram_tensor("stout%d_%d" % (i, k), [GRP * D, 32], F32) for k in range(9)] for i in range(L)]

    with es:
        P = Prog(nc, es)
        arena = es.enter_context(nc.sbuf_tensor("arena", [128, 52736], F32))
        pss = [es.enter_context(nc.psum_tensor("ps%d" % i, [128, 512], F32)) for i in range(8)]
        st = {'off': 0, 'ps': 0, 'tg': 0}

        def alloc(shape, dt):
            n = 1
            for v in shape[1:]:
                n *= v
            words = n if dt == F32 else (n + 1) // 2
            words = (words + 7) // 8 * 8
            off = st['off']
            st['off'] = off + words
            assert st["off"] <= 52736, ("SBUF arena overflow", st['off'])
            v = arena[:, off:off + words]
            if dt == BF16:
                v = v.bitcast(BF16)
            v = v[:, 0:n]
            if len(shape) == 3:
                v = v.rearrange("p (a b) -> p a b", a=shape[1])
            elif len(shape) == 4:
                v = v.rearrange("p (a b c) -> p a b c", a=shape[1], b=shape[2])
            return v

        def nextps():
            i = st['ps']
            st['ps'] = (i + 1) % 8
            return pss[i], 'ps%d' % i

        def MM(out, lhsT, rhs, start, stop, reads, writes):
            P.op('pe', lambda e: e.matmul(out, lhsT=lhsT, rhs=rhs, start=start, stop=stop), reads, writes)

        def ACT(out, in_, func, reads, writes, bias=None, scale=1.0):
            kw = {}
            if bias is not None:
                kw['bias'] = bias
            P.op('act', lambda e: e.activation(out=out, in_=in_, func=func, scale=scale, **kw), reads, writes)

        def TT(eng, out, a, b, op, reads, writes):
            P.op(eng, lambda e: e.tensor_tensor(out=out, in0=a, in1=b, op=op), reads, writes)

        def TS(eng, out, a, s1, s2, op0, op1, reads, writes):
            if op1 is None:
                P.op(eng, lambda e: e.tensor_scalar(out=out, in0=a, scalar1=s1, scalar2=None, op0=op0), reads, writes)
            else:
                P.op(eng, lambda e: e.tensor_scalar(out=out, in0=a, scalar1=s1, scalar2=s2, op0=op0, op1=op1), reads, writes)

        def STT(out, a, s, b, op0, op1, reads, writes):
            P.op('dve', lambda e: e.scalar_tensor_tensor(out=out, in0=a, scalar=s, in1=b, op0=op0, op1=op1), reads, writes)

        def CP(eng, out, in_, reads, writes):
            if eng == 'act':
                P.op('act', lambda e: e.activation(out=out, in_=in_, func=AF.Copy), reads, writes)
            else:
                P.op(eng, lambda e: e.tensor_copy(out=out, in_=in_), reads, writes)

        def DMA(q, out, in_, reads, writes, key):
            P.op(q, lambda e: e.dma_start(out=out, in_=in_), reads, writes, dkey=key)

        def evac_copy(out, in_, reads, writes):
            st['tg'] ^= 1
            CP('act' if st['tg'] else 'dve', out, in_, reads, writes)

        consts = alloc([128, 384], F32)
        ident = consts[:, 0:128]
        tri = consts[:, 128:256]
        ones_f = consts[:, 256:384]
        ones_b = alloc([128, 128], BF16)
        vecs = alloc([128, L * NVL + 8], F32)
        pc = alloc([128, 24], F32)
        Cst = alloc([128, NH, 2, 258], F32)
        Cbf = alloc([128, NH, 2, 258], BF16)
        bacc = alloc([128, 4], F32)
        xh = alloc([128, KC, HB], F32)
        xhn = alloc([128, KC, HB], BF16)
        carry_xm0 = alloc([128, KC, 3], BF16)
        carry_u0 = alloc([128, KC, HB], BF16)
        hsq = alloc([128, KC, HB], BF16)
        hrs = alloc([128, 2, HB], F32)
        nf = alloc([128, 1], F32)
        persist_mark = st['off']

        DMA('sp', consts, cst_in[:, :], [], ['c'], 'c0')
        DMA('sp', vecs, vecs_in[:, :], [], ['c'], 'c0')
        DMA('sp', pc, pc_in[:, :], [], ['c'], 'c0')
        CP('dve', ones_b, ones_f, ['c'], ['c'])
        P.op('dve', lambda e: e.tensor_reduce(out=nf, in_=pc[:, 0:4], axis=mybir.AxisListType.X, op=ALU.add), ['c'], ['c'])
        P.barrier()

        def vcol(l, name, i, n=1):
            o = l * NVL + VOFF[name] + i
            return vecs[:, o:o + n]

        def loadw(dst, src2d, r0, c0, nk, ncols):
            for kc in range(nk):
                for cb in range(0, ncols, 1024):
                    ce = min(ncols, cb + 1024)
                    DMA('pool', dst[:, kc, cb:ce], src2d[r0 + kc * 128:r0 + (kc + 1) * 128, c0 + cb:c0 + ce], [], ['W'], 'w')

        def rms(xv, N, gname, l, outv, rX, rOut, sq, rs, gcol0=None):
            ACT(sq[:, :, 0:N], xv, AF.Square, [rX], ['sq'])
            ps, rp = nextps()
            for c in range(KC):
                MM(ps[:, 0:N], ones_b, sq[:, c, 0:N], c == 0, c == KC - 1, ['sq', 'c'], [rp])
            ACT(rs[:, 0, 0:N], ps[:, 0:N], AF.Sqrt, [rp], ['rs0'], bias=RMS_EPS, scale=1.0 / D)
            P.op('dve', lambda e: e.reciprocal(out=rs[:, 1, 0:N], in_=rs[:, 0, 0:N]), ['rs0'], ['rs1'])
            for c in range(KC):
                g = vcol(l, gname, c) if gcol0 is None else vecs[:, gcol0 + c:gcol0 + c + 1]
                STT(outv[:, c, :], xv[:, c, :], g, rs[:, 1, 0:N], ALU.mult, ALU.mult, [rX, 'rs1', 'c'], [rOut])

        def proj(W, oc, xn, N, rXn):
            ps, rp = nextps()
            for kc in range(KC):
                MM(ps[:, 0:N], W[:, kc, oc * 128:(oc + 1) * 128], xn[:, kc, 0:N], kc == 0, kc == KC - 1, [rXn, 'W'], [rp])
            return ps, rp

        def mkdiag(dg, l, name, nk, nchunk):
            for k in range(nk):
                for c in range(nchunk):
                    st['tg'] ^= 1
                    if st['tg']:
                        TS('dve', dg[:, k, c, :], ident, vcol(l, name, k * nchunk + c), None, ALU.mult, None, ['c'], ['W'])
                    else:
                        TS('pool', dg[:, k, c, :], ident, vcol(l, name, k * nchunk + c), 0.0, ALU.mult, ALU.add, ['c'], ['W'])

        def halo_gather(src, gname, l, idx):
            hin = hins[idx]
            hout = houts[idx]
            hb = alloc([128, KC, HB], F32)
            DMA('sp', hb, src[:, S - HB:S].rearrange("(c p) t -> p c t", p=128), [src.tensor.name], ['hb'], 'hx')
            DMA('sp', hin.ap().rearrange("(c p) t -> p c t", p=128), hb, ['hb'], ['hin'], 'hx')
            P.op('pool', lambda e: e.collective_compute("AllGather", ALU.bypass, replica_groups=[[0, 1, 2, 3], [4, 5, 6, 7]],
                                                        ins=[hin.ap().opt()], outs=[hout.ap().opt()]),
                 ['hin'], ['hout'], dkey='cch%d' % idx, cc=True)
            hg = alloc([128, GRP, KC, HB], F32)
            for r in range(GRP):
                DMA('sp', hg[:, r], hout.ap()[r * D:(r + 1) * D, :].rearrange("(c p) t -> p c t", p=128), ['hout'], ['hg'], 'hx')
            TS('dve', xh, hg[:, 0], pc[:, 0:1], None, ALU.mult, None, ['hg', 'c'], ['xh'])
            for r in range(1, GRP):
                STT(xh, hg[:, r], pc[:, r:r + 1], xh, ALU.mult, ALU.add, ['hg', 'c', 'xh'], ['xh'])
            rms(xh, HB, gname, l, xhn, 'xh', 'xhn', hsq, hrs)

        def chk(label):
            if cfg.stop == label:
                P.muted = True
        cur = xT_in
        for l in (range(L) if cfg.stop != 'INIT' else []):
            last = (l == L - 1)
            w_in_l = w_in[l]
            st['off'] = persist_mark
            halo_gather(cur, 'norm_mix', l, 2 * l)
            P.barrier()
            chk('E1')

            for full in (False, True):
                st['off'] = persist_mark
                ncols = 3072 if full else 1024
                Wi = alloc([128, KC, ncols], BF16)
                BD = alloc([128, 3, KC, 128], BF16)
                WG = alloc([128, 24, 8], BF16)
                dg4 = alloc([128, 4, KC, 128], BF16)
                loadw(Wi[:, :, 0:1024], w_in_l, 0, 0, KC, 1024)
                if full:
                    loadw(Wi[:, :, 1024:2048], w_in_l, 0, 1024, KC, 1024)
                    loadw(Wi[:, :, 2048:3072], w_in_l, 0, 4096, KC, 1024)
                    Wd = alloc([128, KC, D], BF16)
                    loadw(Wd, wmd_in[l], 0, 0, KC, D)
                if not full:
                    Wi2 = alloc([128, KC, 3072], BF16)
                    loadw(Wi2[:, :, 0:2048], w_in_l, 0, 2048, KC, 2048)
                    loadw(Wi2[:, :, 2048:3072], w_in_l, 0, 5120, KC, 1024)
                    sg_ = alloc([128, KC, T], BF16)
                    ub = alloc([128, KC, T], BF16)
                    sgcb = alloc([128, KC, T], BF16)
                    hsg = alloc([128, KC, HB], BF16)
                for i in range(3):
                    DMA('pool', BD[:, i], bd_in[l, i].rearrange("c p d -> p c d"), [], ['W'], 'w')
                DMA('pool', WG.rearrange("p c g -> p (c g)"), wg_in[l], [], ['W'], 'w')
                mkdiag(dg4, l, 'ml_conv_w', 4, KC)
                xt = [alloc([128, KC, T], F32) for _ in range(2)]
                sq = alloc([128, KC, T], BF16)
                rs = alloc([128, 2, T], F32)
                xn = alloc([128, KC, T], BF16)
                xme = [alloc([128, KC, T + 3], BF16) for _ in range(2)]
                xc = alloc([128, KC, T], BF16)
                qT = alloc([128, KC, T], BF16)
                kT = alloc([128, KC, T], BF16)
                vT = alloc([128, KC, T], BF16)
                ktm = [alloc([128, D], BF16) for _ in range(NJ)]
                vtm = [alloc([128, D], BF16) for _ in range(NJ)]
                gsb = alloc([128, 8], F32)
                e1 = alloc([128, 4], F32)
                spl = alloc([128, 4], F32)
                tmp = alloc([128, 4], F32)
                tmp2 = alloc([128, 4], F32)
                wv = alloc([128, 4], F32)
                ebend = alloc([128, 4], F32)
                wk = [alloc([128, DH], BF16) for _ in range(2)]
                if full:
                    siluz = alloc([128, KC, T], BF16)
                    sgm = alloc([128, KC, T], BF16)
                    sxc = alloc([128, KC, T], BF16)
                    eb = alloc([128, 4], F32)
                    av = alloc([128, 4], F32)
                    PT = [alloc([128, 128], BF16) for _ in range(2)]
                    numS = alloc([128, NH, DH], F32)
                    denS = alloc([128, 4], F32)
                    d2 = alloc([128, 4], F32)
                    st6 = alloc([128, NH, 6], F32)
                    mv = alloc([128, NH, 2], F32)
                    vv = alloc([128, 4], F32)
                    rsd = alloc([128, 4], F32)
                    hn = alloc([128, D], F32)
                    o1 = alloc([128, KC, T], BF16)
                    o2 = alloc([128, KC, T], BF16)
                    gy = alloc([128, KC, T], BF16)
                if not full:
                    for c in range(KC):
                        ps, rp = proj(Wi, c, xhn, HB, 'xhn')
                        ACT(hsq[:, c, :], ps[:, 0:HB], AF.Identity, [rp, 'c'], ['hsq'], bias=vcol(l, 'b_in', c))
                    TS('dve', carry_xm0, hsq[:, :, HB - 3:HB], nf, None, ALU.mult, None, ['hsq', 'c'], ['cxm0'])
                    for c in range(KC):
                        ps, rp = proj(Wi2, 8 + c, xhn, HB, 'xhn')
                        ACT(hsg[:, c, :], ps[:, 0:HB], AF.Sigmoid, [rp, 'c'], ['hsg'], bias=vcol(l, 'b_in', 24 + c))
                        ps, rp = proj(Wi2, c, xhn, HB, 'xhn')
                        STT(carry_u0[:, c, :], ps[:, 0:HB], vcol(l, 'b_in', 16 + c), hsg[:, c, :], ALU.add, ALU.mult, [rp, 'hsg', 'c'], ['cu0'])
                    TS('dve', carry_u0, carry_u0, nf, None, ALU.mult, None, ['cu0', 'c'], ['cu0'])
                    P.op('dve', lambda e: e.memset(Cst.rearrange("p h c e -> p (h c e)"), 0.0), [], ['Cst'])
                    P.op('dve', lambda e: e.memset(bacc, 0.0), [], ['bacc'])
                else:
                    for h in range(NH):
                        CP('act', Cbf[:, h, :, 0:257], Cst[:, h, :, 0:257], ['Cst'], ['Cbf%d' % h])
                if not full:
                    chk('P1aW')
                DMA('sp', xt[0], cur[:, 0:T].rearrange("(c p) t -> p c t", p=128), [cur.tensor.name], ['xt0'], 'xt0')
                for it in range(NT):
                    par = it % 2
                    t0 = it * T
                    if it + 1 < NT:
                        DMA('sp', xt[1 - par], cur[:, t0 + T:t0 + 2 * T].rearrange("(c p) t -> p c t", p=128),
                            [cur.tensor.name], ['xt%d' % (1 - par)], 'xt%d' % (1 - par))
                    rX = 'xt%d' % par
                    rms(xt[par], T, 'norm_mix', l, xn, rX, 'xn', sq, rs)
                    X = xme[par]
                    rXm = 'xme%d' % par
                    if it == 0:
                        CP('pool', X[:, :, 0:3], carry_xm0, ['cxm0'], [rXm])
                    else:
                        CP('pool', X[:, :, 0:3], xme[1 - par][:, :, T:T + 3], ['xme%d' % (1 - par)], [rXm])
                    for c in range(KC):
                        ps, rp = proj(Wi, c, xn, T, 'xn')
                        ACT(X[:, c, 3:3 + T], ps[:, 0:T], AF.Identity, [rp, 'c'], [rXm], bias=vcol(l, 'b_in', c))
                    if full:
                        for c in range(KC):
                            ps, rp = proj(Wi, 8 + c, xn, T, 'xn')
                            ACT(siluz[:, c, :], ps[:, 0:T], AF.Silu, [rp, 'c'], ['siluz'], bias=vcol(l, 'b_in', 8 + c))
                        for c in range(KC):
                            ps, rp = proj(Wi, 16 + c, xn, T, 'xn')
                            ACT(sgm[:, c, :], ps[:, 0:T], AF.Sigmoid, [rp, 'c'], ['sgm'], bias=vcol(l, 'b_in', 32 + c))
                    if not full:
                        for c in range(KC):
                            ps, rp = proj(Wi2, 8 + c, xn, T, 'xn')
                            ACT(sg_[:, c, :], ps[:, 0:T], AF.Sigmoid, [rp, 'c'], ['sg_'], bias=vcol(l, 'b_in', 24 + c))
                            ps, rp = proj(Wi2, c, xn, T, 'xn')
                            STT(ub[:, c, :], ps[:, 0:T], vcol(l, 'b_in', 16 + c), sg_[:, c, :], ALU.add, ALU.mult, [rp, 'sg_', 'c'], ['ub'])
                        DMA('sp', UB[:, t0:t0 + T].rearrange("(c p) t -> p c t", p=128), ub, ['ub'], ['UB'], 'ubst')
                        for c in range(KC):
                            ps, rp = proj(Wi2, 16 + c, xn, T, 'xn')
                            ACT(sgcb[:, c, :], ps[:, 0:T], AF.Sigmoid, [rp, 'c'], ['sgcb'], bias=vcol(l, 'b_in', 40 + c))
                        DMA('sp', SGCB[:, t0:t0 + T].rearrange("(c p) t -> p c t", p=128), sgcb, ['sgcb'], ['SGCB'], 'sgst')
                    for c in range(KC):
                        ps, rp = nextps()
                        for k in range(4):
                            MM(ps[:, 0:T], dg4[:, k, c, :], X[:, c, k:k + T], k == 0, k == 3, [rXm, 'W'], [rp])
                        ACT(xc[:, c, :], ps[:, 0:T], AF.Silu, [rp, 'c'], ['xc'], bias=vcol(l, 'ml_conv_b', c))
                    if full:
                        for c in range(KC):
                            TS('pool', sxc[:, c, :], xc[:, c, :], vcol(l, 'ml_skip', c), 0.0, ALU.mult, ALU.add, ['xc', 'c'], ['sxc'])
                    for i, (dst, rd) in enumerate(((qT, 'qT'), (kT, 'kT'), (vT, 'vT'))):
                        for c in range(KC):
                            ps, rp = nextps()
                            src = X[:, c, 3:3 + T] if i == 2 else xc[:, c, :]
                            MM(ps[:, 0:T], BD[:, i, c, :], src, True, True, ['xc', rXm, 'W'], [rp])
                            evac_copy(dst[:, c, :], ps[:, 0:T], [rp], [rd])
                    for j in range(NJ):
                        js = slice(j * 128, (j + 1) * 128)
                        for i, (dst, rd) in enumerate(((ktm[j], 'ktm%d' % j), (vtm[j], 'vtm%d' % j))):
                            for hf in range(2):
                                ps, rp = nextps()
                                for cc in range(4):
                                    c = hf * 4 + cc
                                    src = X[:, c, 3 + j * 128:3 + (j + 1) * 128] if i == 1 else xc[:, c, js]
                                    MM(ps[:, cc * 128:(cc + 1) * 128], src, BD[:, 1 + i, c, :], True, True, ['xc', rXm, 'W'], [rp])
                                evac_copy(dst[:, hf * 512:(hf + 1) * 512], ps[:, 0:512], [rp], [rd])
                        ps, rp = nextps()
                        n = 0
                        for i, (srcT, rd) in enumerate(((qT, 'qT'), (kT, 'kT'), (vT, 'vT'))):
                            for c in range(KC):
                                MM(ps[:, 0:8], srcT[:, c, js], WG[:, i * 8 + c, :], n == 0, n == 23, [rd, 'W'], [rp])
                                n += 1
                        TT('dve', gsb, ps[:, 0:8], vcol(l, 'b_gate', 0, 8), ALU.add, [rp, 'c'], ['gsb'])
                        ACT(e1, gsb[:, 4:8], AF.Exp, ['gsb'], ['e1'], scale=-1.0)
                        ACT(spl, e1, AF.Ln, ['e1'], ['spl'], bias=1.0)
                        psg, rpg = nextps()
                        MM(psg[:, 0:4], tri, spl, True, True, ['spl', 'c'], [rpg])
                        MM(psg[:, 4:8], ones_f, spl, True, True, ['spl', 'c'], [rpg])
                        TT('dve', tmp, psg[:, 0:4], gsb[:, 0:4], ALU.add, [rpg, 'gsb'], ['tmp'])
                        TT('dve', tmp2, tmp, psg[:, 4:8], ALU.subtract, [rpg, 'tmp'], ['tmp2'])
                        ACT(wv, tmp2, AF.Exp, ['tmp2'], ['wv'], bias=-LN16)
                        ACT(ebend, psg[:, 4:8], AF.Exp, [rpg], ['ebend'], scale=-1.0)
                        TT('dve', bacc, bacc, psg[:, 4:8], ALU.add, [rpg, 'bacc'], ['bacc'])
                        if full:
                            ACT(eb, psg[:, 0:4], AF.Exp, [rpg], ['eb'], scale=-1.0)
                            ACT(av, tmp, AF.Exp, ['tmp'], ['av'], bias=-LN16)
                            psD, rpD = nextps()
                            psH = [nextps(), nextps()]
                        for h in range(NH):
                            hs = slice(h * DH, (h + 1) * DH)
                            if full:
                                psS, rpS = nextps()
                                for dc in range(2):
                                    MM(psS[:, 0:128], kT[:, h * 2 + dc, js], qT[:, h * 2 + dc, js], dc == 0, dc == 1, ['kT', 'qT'], [rpS])
                                pt = PT[h % 2]
                                rpt = 'PT%d' % (h % 2)
                                STT(pt, psS[:, 0:128], av[:, h:h + 1], tri, ALU.mult, ALU.mult, [rpS, 'av', 'c'], [rpt])
                                pH, rpH = psH[h // 2]
                                ho = (h % 2) * 256
                                MM(pH[:, ho:ho + 256], pt, vtm[j][:, hs], True, False, [rpt, 'vtm%d' % j], [rpH])
                                for dc in range(2):
                                    MM(pH[:, ho:ho + 256], qT[:, h * 2 + dc, js], Cbf[:, h, dc, 0:256], False, dc == 1,
                                       ['qT', 'Cbf%d' % h], [rpH])
                                MM(psD[:, h:h + 1], pt, ones_b[:, 0:1], True, False, [rpt, 'c'], [rpD])
                                for dc in range(2):
                                    MM(psD[:, h:h + 1], qT[:, h * 2 + dc, js], Cbf[:, h, dc, 256:257], False, dc == 1,
                                       ['qT', 'Cbf%d' % h], [rpD])
                                TS('dve', numS[:, h, :], pH[:, ho:ho + 256], eb[:, h:h + 1], None, ALU.mult, None, [rpH, 'eb'], ['numS%d' % h])
                            wkb = wk[h % 2]
                            rwk = 'wk%d' % (h % 2)
                            TS('dve', wkb, ktm[j][:, hs], wv[:, h:h + 1], None, ALU.mult, None, ['ktm%d' % j, 'wv'], [rwk])
                            psC, rpC = nextps()
                            psN, rpN = nextps()
                            for dc in range(2):
                                MM(psC[:, dc * 256:(dc + 1) * 256], wkb[:, dc * 128:(dc + 1) * 128], vtm[j][:, hs], True, True,
                                   [rwk, 'vtm%d' % j], [rpC])
                                MM(psN[:, dc:dc + 1], wkb[:, dc * 128:(dc + 1) * 128], ones_b[:, 0:1], True, True, [rwk, 'c'], [rpN])
                            rC = ['Cst', 'Cbf%d' % h] if full else ['Cst']
                            STT(Cst[:, h, :, 0:256], Cst[:, h, :, 0:256], ebend[:, h:h + 1],
                                psC[:, 0:512].rearrange("p (a b) -> p a b", a=2), ALU.mult, ALU.add, [rpC, 'ebend'] + rC, ['Cst'])
                            STT(Cst[:, h, :, 256:257], Cst[:, h, :, 256:257], ebend[:, h:h + 1],
                                psN[:, 0:2].rearrange("p (a b) -> p a b", a=2), ALU.mult, ALU.add, [rpN, 'ebend'] + rC, ['Cst'])
                            if full:
                                CP('act', Cbf[:, h, :, 0:257], Cst[:, h, :, 0:257], ['Cst'], ['Cbf%d' % h])
                        if full:
                            rN = ['numS%d' % h for h in range(NH)]
                            TT('dve', denS, psD[:, 0:4], eb, ALU.mult, [rpD, 'eb'], ['denS'])
                            TT('dve', d2, denS, denS, ALU.mult, ['denS'], ['d2'])
                            TS('dve', d2, d2, 1.0, LN_EPS, ALU.max, ALU.mult, ['d2'], ['d2'])
                            for h in range(NH):
                                P.op('dve', (lambda h: lambda e: e.bn_stats(out=st6[:, h, :], in_=numS[:, h, :]))(h), rN, ['st6'])
                            for h in range(NH):
                                P.op('dve', (lambda h: lambda e: e.bn_aggr(out=mv[:, h, :], in_=st6[:, h, :]))(h), ['st6'], ['mv'])
                            TT('dve', vv, mv[:, :, 1], d2, ALU.add, ['mv', 'd2'], ['vv'])
                            ACT(vv, vv, AF.Sqrt, ['vv'], ['vv'])
                            P.op('dve', lambda e: e.reciprocal(out=rsd, in_=vv), ['vv'], ['rsd'])
                            for h in range(NH):
                                TS('dve', hn[:, h * DH:(h + 1) * DH], numS[:, h, :], mv[:, h, 0:1], rsd[:, h:h + 1], ALU.subtract, ALU.mult,
                                   rN + ['mv', 'rsd'], ['hn'])
                            for hf in range(2):
                                ps, rp = nextps()
                                for cc in range(4):
                                    c = hf * 4 + cc
                                    P.op('pe', (lambda ps, cc, c: lambda e: e.transpose(ps[:, cc * 128:(cc + 1) * 128], hn[:, c * 128:(c + 1) * 128], ident))(ps, cc, c),
                                         ['hn', 'c'], [rp])
                                for cc in range(4):
                                    c = hf * 4 + cc
                                    STT(o1[:, c, js], ps[:, cc * 128:(cc + 1) * 128], vcol(l, 'ml_norm_g', c), sxc[:, c, js], ALU.mult, ALU.add,
                                        [rp, 'sxc', 'c'], ['o1'])
                    if full:
                        TT('pool', o2, o1, siluz, ALU.mult, ['o1', 'siluz'], ['o2'])
                        for oc in range(KC):
                            ps, rp = proj(Wd, oc, o2, T, 'o2')
                            TT('dve', gy[:, oc, :], ps[:, 0:T], sgm[:, oc, :], ALU.mult, [rp, 'sgm'], ['gy'])
                        DMA('sp', GY[:, t0:t0 + T].rearrange("(c p) t -> p c t", p=128), gy, ['gy'], ['GY'], 'gyst')
                if not full:
                    P.barrier()
                    chk('P1aT')
                    st['off'] = persist_mark
                    stin = stins[l]
                    stout = stouts[l]
                    for h in range(NH):
                        for dc in range(2):
                            TS('dve', Cst[:, h, dc, 257:258], bacc[:, h:h + 1], -1.0, None, ALU.mult, None, ['bacc', 'Cst'], ['Cst'])
                    sg = alloc([128, GRP, 8, 258], F32)
                    Cv0 = Cst.rearrange("p h c e -> p (h c) e")
                    for k in range(9):
                        c0 = k * 32
                        c1 = min(258, c0 + 32)
                        w_ = c1 - c0
                        DMA('sp', stin[k].ap().rearrange("(g p) e -> p g e", p=128)[:, :, 0:w_], Cv0[:, :, c0:c1], ['Cst'], ['stin%d' % k], 'sx')
                        P.op('pool', lambda e, a=stin[k], b=stout[k]: e.collective_compute("AllGather", ALU.bypass, replica_groups=[[0, 1, 2, 3], [4, 5, 6, 7]],
                                                                              ins=[a.ap().opt()], outs=[b.ap().opt()]),
                             ['stin%d' % k], ['stout%d' % k], dkey='ccs%d_%d' % (l, k), cc=True)
                        for r in range(GRP):
                            DMA('sp', sg[:, r, :, c0:c1], stout[k].ap()[r * D:(r + 1) * D, :].rearrange("(g p) e -> p g e", p=128)[:, :, 0:w_],
                                ['stout%d' % k], ['sg'], 'sx')
                    Ew = alloc([128, GRP, 8], F32)
                    for i in range(GRP):
                        TS('dve', Ew[:, i, :], sg[:, 0, :, 257], pc[:, 4 + i * 4:5 + i * 4], None, ALU.mult, None, ['sg', 'c'], ['Ew'])
                        for m in range(1, GRP):
                            STT(Ew[:, i, :], sg[:, m, :, 257], pc[:, 4 + i * 4 + m:5 + i * 4 + m], Ew[:, i, :], ALU.mult, ALU.add,
                                ['sg', 'c', 'Ew'], ['Ew'])
                    ACT(Ew, Ew, AF.Exp, ['Ew'], ['Ew'])
                    for i in range(GRP):
                        TS('dve', Ew[:, i, :], Ew[:, i, :], pc[:, 20 + i:21 + i], None, ALU.mult, None, ['Ew', 'c'], ['Ew'])
                    Cv = Cst.rearrange("p h c e -> p (h c) e")
                    for g in range(8):
                        TS('dve', Cv[:, g, 0:257], sg[:, 0, g, 0:257], Ew[:, 0, g:g + 1], None, ALU.mult, None, ['sg', 'Ew', 'Cst'], ['Cst'])
                        for i in range(1, GRP):
                            STT(Cv[:, g, 0:257], sg[:, i, g, 0:257], Ew[:, i, g:g + 1], Cv[:, g, 0:257], ALU.mult, ALU.add,
                                ['sg', 'Ew', 'Cst'], ['Cst'])
                P.barrier()
                chk('E2' if not full else 'P1b')

            st['off'] = persist_mark
            Wpw = alloc([128, KC, D], BF16)
            loadw(Wpw, wpw_in[l], 0, 0, KC, D)
            Wo = alloc([128, KC, D], BF16)
            loadw(Wo, wout_in[l], 0, 0, KC, D)
            dg31 = alloc([128, 31, KC, 128], BF16)
            mkdiag(dg31, l, 'cv_dw_w', 31, KC)
            xt = [alloc([128, KC, T], F32) for _ in range(2)]
            gyt = [alloc([128, KC, T], BF16) for _ in range(2)]
            sgc = [alloc([128, KC, T], BF16) for _ in range(2)]
            ue = [alloc([128, KC, T + 30], BF16) for _ in range(2)]
            cvb = alloc([128, KC, T], BF16)
            sqb = alloc([128, KC, T], BF16)
            mean = alloc([128, T], F32)
            m2 = alloc([128, T], F32)
            var = alloc([128, T], F32)
            rstd = alloc([128, T], F32)
            t1 = alloc([128, KC, T], F32)
            vact = alloc([128, KC, T], BF16)
            mg = alloc([128, KC, T], BF16)
            x1 = alloc([128, KC, T], F32)

            def p1c_loads(it):
                par = it % 2
                t0 = it * T
                DMA('sp', xt[par], cur[:, t0:t0 + T].rearrange("(c p) t -> p c t", p=128), [cur.tensor.name], ['xt%d' % par], 'xt%d' % par)
                DMA('sp', gyt[par], GY[:, t0:t0 + T].rearrange("(c p) t -> p c t", p=128), ['GY'], ['gyt%d' % par], 'gyt%d' % par)
                DMA('sp', sgc[par], SGCB[:, t0:t0 + T].rearrange("(c p) t -> p c t", p=128), ['SGCB'], ['sgc%d' % par], 'sgc%d' % par)
                if it == 0:
                    DMA('sp', ue[par][:, :, 30:30 + T], UB[:, 0:T].rearrange("(c p) t -> p c t", p=128), ['UB'], ['ue%d' % par], 'ue%d' % par)
                    CP('pool', ue[par][:, :, 0:30], carry_u0[:, :, HB - 30:HB], ['cu0'], ['ue%d' % par])
                else:
                    DMA('sp', ue[par], UB[:, t0 - 30:t0 + T].rearrange("(c p) t -> p c t", p=128), ['UB'], ['ue%d' % par], 'ue%d' % par)
            p1c_loads(0)
            for it in range(NT):
                par = it % 2
                t0 = it * T
                if it + 1 < NT:
                    p1c_loads(it + 1)
                rX = 'xt%d' % par
                U = ue[par]
                rU = 'ue%d' % par
                for c in range(KC):
                    ps, rp = nextps()
                    for k in range(31):
                        MM(ps[:, 0:T], dg31[:, k, c, :], U[:, c, k:k + T], k == 0, k == 30, [rU, 'W'], [rp])
                    ACT(cvb[:, c, :], ps[:, 0:T], AF.Identity, [rp, 'c'], ['cvb'], bias=vcol(l, 'cv_dw_b', c))
                    ACT(sqb[:, c, :], ps[:, 0:T], AF.Square, [rp, 'c'], ['sqb'], bias=vcol(l, 'cv_dw_b', c))
                psM, rpM = nextps()
                psQ, rpQ = nextps()
                for c in range(KC):
                    MM(psM[:, 0:T], ones_b, cvb[:, c, :], c == 0, c == KC - 1, ['cvb', 'c'], [rpM])
                for c in range(KC):
                    MM(psQ[:, 0:T], ones_b, sqb[:, c, :], c == 0, c == KC - 1, ['sqb', 'c'], [rpQ])
                TS('dve', mean, psM[:, 0:T], 1.0 / D, None, ALU.mult, None, [rpM], ['mean'])
                TT('dve', m2, mean, mean, ALU.mult, ['mean'], ['m2'])
                STT(var, psQ[:, 0:T], 1.0 / D, m2, ALU.mult, ALU.subtract, [rpQ, 'm2'], ['var'])
                ACT(var, var, AF.Sqrt, ['var'], ['var'], bias=LN_EPS)
                P.op('dve', lambda e: e.reciprocal(out=rstd, in_=var), ['var'], ['rstd'])
                for c in range(KC):
                    rt = 't1_%d' % c
                    TT('pool', t1[:, c, :], cvb[:, c, :], mean, ALU.subtract, ['cvb', 'mean'], [rt])
                    TT('dve', t1[:, c, :], t1[:, c, :], rstd, ALU.mult, [rt, 'rstd'], [rt])
                    ACT(vact[:, c, :], t1[:, c, :], AF.Silu, [rt, 'c'], ['vact'], bias=vcol(l, 'cv_ln_b', c), scale=vcol(l, 'cv_ln_g', c))
                for oc in range(KC):
                    ps, rp = proj(Wpw, oc, vact, T, 'vact')
                    STT(mg[:, oc, :], ps[:, 0:T], vcol(l, 'cv_b_pw', oc), sgc[par][:, oc, :], ALU.add, ALU.mult, [rp, 'sgc%d' % par, 'c'], ['mg'])
                TT('pool', mg, mg, gyt[par], ALU.add, ['mg', 'gyt%d' % par], ['mg'])
                for oc in range(KC):
                    ps, rp = proj(Wo, oc, mg, T, 'mg')
                    TT('dve', x1[:, oc, :], ps[:, 0:T], xt[par][:, oc, :], ALU.add, [rp, rX], ['x1'])
                DMA('sp', D1[:, t0:t0 + T].rearrange("(c p) t -> p c t", p=128), x1, ['x1'], ['D1'], 'xst')
            P.barrier()
            chk('P1c')

            st['off'] = persist_mark
            Wq = alloc([128, KC, D], BF16)
            Wk = alloc([128, KC, D], BF16)
            Wv = alloc([128, KC, D], BF16)
            Wxo = alloc([128, KC, D], BF16)
            loadw(Wq, xwq_in[l], 0, 0, KC, D)
            loadw(Wk, xwk_in[l], 0, 0, KC, D)
            loadw(Wv, xwv_in[l], 0, 0, KC, D)
            loadw(Wxo, xwo_in[l], 0, 0, KC, D)
            memt = alloc([128, KC, NMEM], F32)
            memn = alloc([128, KC, NMEM], BF16)
            KT = alloc([128, KC, NMEM], BF16)
            Vt = alloc([128, 2, D], BF16)
            xt = [alloc([128, KC, T], F32) for _ in range(2)]
            sq = alloc([128, KC, T], BF16)
            rs = alloc([128, 2, T], F32)
            xn = alloc([128, KC, T], BF16)
            qx = alloc([128, KC, T], BF16)
            pT = [[alloc([128, T], BF16) for _ in range(2)] for _ in range(2)]
            rz = alloc([128, T], F32)
            ox = alloc([128, KC, T], BF16)
            x2 = alloc([128, KC, T], F32)
            DMA('sp', memt, memT_in.rearrange("(c p) t -> p c t", p=128), [], ['memt'], 'memt')
            rms(memt, NMEM, 'norm_mem', l, memn, 'memt', 'memn', sq, rs)
            for oc in range(KC):
                ps, rp = proj(Wk, oc, memn, NMEM, 'memn')
                evac_copy(KT[:, oc, :], ps[:, 0:NMEM], [rp], ['KT'])
            for ncn in range(2):
                for hf in range(2):
                    ps, rp = nextps()
                    for kc in range(KC):
                        MM(ps[:, 0:512], memn[:, kc, ncn * 128:(ncn + 1) * 128], Wv[:, kc, hf * 512:(hf + 1) * 512], kc == 0, kc == KC - 1,
                           ['memn', 'W'], [rp])
                    evac_copy(Vt[:, ncn, hf * 512:(hf + 1) * 512], ps[:, 0:512], [rp], ['Vt'])
            DMA('sp', xt[0], D1[:, 0:T].rearrange("(c p) t -> p c t", p=128), ['D1'], ['xt0'], 'xt0')
            for it in range(NT):
                par = it % 2
                t0 = it * T
                if it + 1 < NT:
                    DMA('sp', xt[1 - par], D1[:, t0 + T:t0 + 2 * T].rearrange("(c p) t -> p c t", p=128),
                        ['D1'], ['xt%d' % (1 - par)], 'xt%d' % (1 - par))
                rX = 'xt%d' % par
                rms(xt[par], T, 'norm_x', l, xn, rX, 'xn', sq, rs)
                for oc in range(KC):
                    ps, rp = proj(Wq, oc, xn, T, 'xn')
                    evac_copy(qx[:, oc, :], ps[:, 0:T], [rp], ['qx'])
                for h in range(NH):
                    pp = pT[h % 2]
                    rpp = 'pT%d' % (h % 2)
                    for ncn in range(2):
                        ps, rp = nextps()
                        for dc in range(2):
                            MM(ps[:, 0:T], KT[:, h * 2 + dc, ncn * 128:(ncn + 1) * 128], qx[:, h * 2 + dc, :], dc == 0, dc == 1, ['KT', 'qx'], [rp])
                        ACT(pp[ncn], ps[:, 0:T], AF.Exp, [rp], [rpp + 'n%d' % ncn], scale=1.0 / 16.0)
                    rpb = [rpp + 'n0', rpp + 'n1']
                    psZ, rpZ = nextps()
                    for ncn in range(2):
                        MM(psZ[:, 0:T], ones_b, pp[ncn], ncn == 0, ncn == 1, rpb + ['c'], [rpZ])
                    P.op('dve', (lambda psZ: lambda e: e.reciprocal(out=rz, in_=psZ[:, 0:T]))(psZ), [rpZ], ['rz'])
                    for dvc in range(2):
                        ps, rp = nextps()
                        for ncn in range(2):
                            MM(ps[:, 0:T], Vt[:, ncn, h * DH + dvc * 128:h * DH + (dvc + 1) * 128], pp[ncn], ncn == 0, ncn == 1, rpb + ['Vt'], [rp])
                        TT('dve', ox[:, h * 2 + dvc, :], ps[:, 0:T], rz, ALU.mult, [rp, 'rz'], ['ox'])
                for oc in range(KC):
                    ps, rp = proj(Wxo, oc, ox, T, 'ox')
                    TT('dve', x2[:, oc, :], ps[:, 0:T], xt[par][:, oc, :], ALU.add, [rp, rX], ['x2'])
                DMA('sp', D2[:, t0:t0 + T].rearrange("(c p) t -> p c t", p=128), x2, ['x2'], ['D2'], 'xst')
            P.barrier()
            chk('XA')

            st['off'] = persist_mark
            halo_gather(D2, 'norm_ffn', l, 2 * l + 1)
            P.barrier()

            for hf in range(2):
                st['off'] = persist_mark
                Wu = alloc([128, KC, 2816], BF16)
                loadw(Wu[:, :, 0:1408], wup_in[l], 0, hf * 1408, KC, 1408)
                loadw(Wu[:, :, 1408:2816], wup_in[l], 0, DFF + hf * 1408, KC, 1408)
                Wdn = alloc([128, 11, D], BF16)
                loadw(Wdn, wdn_in[l], hf * 1408, 0, 11, D)
                dg3 = alloc([128, 3, 22, 128], BF16)

                def fcidx(i):
                    return hf * 11 + i if i < 11 else 22 + hf * 11 + (i - 11)
                for k in range(3):
                    for i in range(22):
                        st['tg'] ^= 1
                        if st['tg']:
                            TS('dve', dg3[:, k, i, :], ident, vcol(l, 'ffn_dw_w', k * FC + fcidx(i)), None, ALU.mult, None, ['c'], ['W'])
                        else:
                            TS('pool', dg3[:, k, i, :], ident, vcol(l, 'ffn_dw_w', k * FC + fcidx(i)), 0.0, ALU.mult, ALU.add, ['c'], ['W'])
                xt = [alloc([128, KC, T], F32) for _ in range(2)]
                xp = [alloc([128, KC, T], F32) for _ in range(2)] if hf == 1 else None
                sq = alloc([128, KC, T], BF16)
                rs = alloc([128, 2, T], F32)
                xn = alloc([128, KC, T], BF16)
                upe = [alloc([128, 22, T + 2], BF16) for _ in range(2)]
                sgt = alloc([128, 11, T], BF16)
                hmid = alloc([128, 11, T], BF16)
                x3 = alloc([128, KC, T], F32)
                carry_f0 = alloc([128, 22, HB], BF16)
                if last and hf == 1:
                    fsq = alloc([128, KC, T], BF16)
                    yo = alloc([128, KC, T], F32)
                for i in range(22):
                    ps, rp = proj(Wu, i, xhn, HB, 'xhn')
                    ACT(carry_f0[:, i, :], ps[:, 0:HB], AF.Identity, [rp, 'c'], ['cf0'], bias=vcol(l, 'ffn_b_up', fcidx(i)))
                TS('dve', carry_f0, carry_f0, nf, None, ALU.mult, None, ['cf0', 'c'], ['cf0'])
                dst = D3 if hf == 0 else (outT if last else D4)
                DMA('sp', xt[0], D2[:, 0:T].rearrange("(c p) t -> p c t", p=128), ['D2'], ['xt0'], 'xt0')
                if hf == 1:
                    DMA('sp', xp[0], D3[:, 0:T].rearrange("(c p) t -> p c t", p=128), ['D3'], ['xp0'], 'xp0')
                for it in range(NT):
                    par = it % 2
                    t0 = it * T
                    if it + 1 < NT:
                        DMA('sp', xt[1 - par], D2[:, t0 + T:t0 + 2 * T].rearrange("(c p) t -> p c t", p=128),
                            ['D2'], ['xt%d' % (1 - par)], 'xt%d' % (1 - par))
                        if hf == 1:
                            DMA('sp', xp[1 - par], D3[:, t0 + T:t0 + 2 * T].rearrange("(c p) t -> p c t", p=128),
                                ['D3'], ['xp%d' % (1 - par)], 'xp%d' % (1 - par))
                    rX = 'xt%d' % par
                    rms(xt[par], T, 'norm_ffn', l, xn, rX, 'xn', sq, rs)
                    U = upe[par]
                    rU = 'upe%d' % par
                    if it == 0:
                        CP('pool', U[:, :, 0:2], carry_f0[:, :, HB - 2:HB], ['cf0'], [rU])
                    else:
                        CP('pool', U[:, :, 0:2], upe[1 - par][:, :, T:T + 2], ['upe%d' % (1 - par)], [rU])
                    for i in range(22):
                        ps, rp = proj(Wu, i, xn, T, 'xn')
                        ACT(U[:, i, 2:2 + T], ps[:, 0:T], AF.Identity, [rp, 'c'], [rU], bias=vcol(l, 'ffn_b_up', fcidx(i)))
                    for i in range(22):
                        ps, rp = nextps()
                        for k in range(3):
                            MM(ps[:, 0:T], dg3[:, k, i, :], U[:, i, k:k + T], k == 0, k == 2, [rU, 'W'], [rp])
                        if i < 11:
                            ACT(sgt[:, i, :], ps[:, 0:T], AF.Silu, [rp, 'c'], ['sgt'], bias=vcol(l, 'ffn_dw_b', fcidx(i)))
                        else:
                            STT(hmid[:, i - 11, :], ps[:, 0:T], vcol(l, 'ffn_dw_b', fcidx(i)), sgt[:, i - 11, :], ALU.add, ALU.mult,
                                [rp, 'sgt', 'c'], ['hmid'])
                    for oc in range(KC):
                        ps, rp = nextps()
                        for kc in range(11):
                            MM(ps[:, 0:T], Wdn[:, kc, oc * 128:(oc + 1) * 128], hmid[:, kc, :], kc == 0, kc == 10, ['hmid', 'W'], [rp])
                        if hf == 0:
                            TT('dve', x3[:, oc, :], ps[:, 0:T], xt[par][:, oc, :], ALU.add, [rp, rX], ['x3'])
                        else:
                            TT('dve', x3[:, oc, :], ps[:, 0:T], xp[par][:, oc, :], ALU.add, [rp, 'xp%d' % par], ['x3'])
                    if last and hf == 1:
                        rms(x3, T, None, l, yo, 'x3', 'yo', fsq, rs, gcol0=L * NVL)
                        DMA('sp', dst[:, t0:t0 + T].rearrange("(c p) t -> p c t", p=128), yo, ['yo'], [dst.tensor.name], 'xst')
                    else:
                        DMA('sp', dst[:, t0:t0 + T].rearrange("(c p) t -> p c t", p=128), x3, ['x3'], [dst.tensor.name], 'xst')
                P.barrier()
            cur = D4
        P.emit(final_keys=['xst'])
    return nc


def colvec(v):
    v = np.asarray(v, np.float32)
    return np.ascontiguousarray(v.reshape(-1, 128).T)


def prep_inputs(inp, cfg):
    L, S = cfg.L, cfg.S
    x = np.asarray(inp['x'], np.float32)
    mem = np.asarray(inp['mem'], np.float32)
    B = x.shape[0]
    assert B * GRP == NCORE and x.shape[1] == GRP * S
    vec_list = []
    for l in range(L):
        cols = []
        for name, w in VEC_LAYOUT:
            if name == 'b_gate':
                cols.append(np.broadcast_to(np.asarray(inp['ml_b_gate'], np.float32)[l][None, :], (128, 8)))
            elif name in ('ml_conv_w', 'cv_dw_w', 'ffn_dw_w'):
                a = np.asarray(inp[name], np.float32)[l]
                cols.append(np.concatenate([colvec(a[k]) for k in range(a.shape[0])], axis=1))
            else:
                cols.append(colvec(np.asarray(inp[name], np.float32)[l]))
            assert cols[-1].shape == (128, w), (name, cols[-1].shape)
        vec_list.append(np.concatenate(cols, axis=1))
    vec_list.append(colvec(inp['final_norm']))
    vecs = np.ascontiguousarray(np.concatenate(vec_list, axis=1), np.float32)
    bd = np.zeros((L, 3, KC, 128, 128), np.float32)
    for i, nm in enumerate(('ml_wq', 'ml_wk', 'ml_wv')):
        w = np.asarray(inp[nm], np.float32)[:L]
        wb = w.reshape(L, KC, 32, 4, 4)
        for n in range(32):
            bd[:, i, :, n * 4:(n + 1) * 4, n * 4:(n + 1) * 4] = wb[:, :, n]
    consts = np.zeros((128, 384), np.float32)
    consts[:, 0:128] = np.eye(128, dtype=np.float32)
    consts[:, 128:256] = np.triu(np.ones((128, 128), np.float32))
    consts[:, 256:384] = 1.0
    shared = {
        'w_in': np.ascontiguousarray(np.asarray(inp['w_in'], np.float32)[:L]),
        'bd': bd,
        'w_gate': np.ascontiguousarray(np.asarray(inp['ml_w_gate'], np.float32)[:L].reshape(L, 24, 128, 8).transpose(0, 2, 1, 3).reshape(L, 128, 192)),
        'ml_w_down': np.ascontiguousarray(np.asarray(inp['ml_w_down'], np.float32)[:L]),
        'cv_w_pw': np.ascontiguousarray(np.asarray(inp['cv_w_pw'], np.float32)[:L]),
        'w_out': np.ascontiguousarray(np.asarray(inp['w_out'], np.float32)[:L]),
        'xa_wq': np.ascontiguousarray(np.asarray(inp['xa_wq'], np.float32)[:L]),
        'xa_wk': np.ascontiguousarray(np.asarray(inp['xa_wk'], np.float32)[:L]),
        'xa_wv': np.ascontiguousarray(np.asarray(inp['xa_wv'], np.float32)[:L]),
        'xa_wo': np.ascontiguousarray(np.asarray(inp['xa_wo'], np.float32)[:L]),
        'ffn_w_up': np.ascontiguousarray(np.asarray(inp['ffn_w_up'], np.float32)[:L]),
        'ffn_w_down': np.ascontiguousarray(np.asarray(inp['ffn_w_down'], np.float32)[:L]),
        'vecs': vecs,
        'consts': consts,
    }
    in_maps = []
    for core in range(NCORE):
        b, r = divmod(core, GRP)
        pcv = np.zeros((24,), np.float32)
        if r > 0:
            pcv[r - 1] = 1.0
        for i in range(GRP):
            for m in range(GRP):
                if i < m < r:
                    pcv[4 + i * 4 + m] = 1.0
            if i < r:
                pcv[20 + i] = 1.0
        m = dict(shared)
        m['xT'] = np.ascontiguousarray(x[b, r * S:(r + 1) * S, :].T)
        m['memT'] = np.ascontiguousarray(mem[b].T)
        m['pc'] = np.ascontiguousarray(np.broadcast_to(pcv[None, :], (128, 24)))
        in_maps.append(m)
    return in_maps


_NC_CACHE = {}


def run(inp, cfg, keys=('outT',)):
    key = (cfg.S, cfg.T, cfg.L, cfg.debug)
    if key not in _NC_CACHE:
        _NC_CACHE[key] = build(cfg)
    nc = _NC_CACHE[key]
    in_maps = prep_inputs(inp, cfg)
    res = run_bass_kernel_spmd(nc, in_maps, core_ids=list(range(NCORE)))
    outs = {}
    B = NCORE // GRP
    for k in keys:
        full = np.zeros((B, GRP * cfg.S, D), np.float32)
        for core in range(NCORE):
            b, r = divmod(core, GRP)
            full[b, r * cfg.S:(r + 1) * cfg.S, :] = np.asarray(res.results[core][k]).astype(np.float32).T
        outs[k] = full
    return outs


def kernel(**inputs):
    cfg = Cfg(S=4096, T=256, L=2, debug=False)
    return run(inputs, cfg)['outT']
```
